# Optimizing a Trainium2 kernel written in Bass

```python
import jax
import jax.numpy as jnp
from jax import lax
import numpy as np

D_MODEL = 2048
BATCH = 2
SEQ = 8192
DEPTH = 4

HEAD_DIM = 128
ROPE_THETA = 10000.0
DSA_HEADS = 8
MOBA_HEADS = 8
IDX_HEADS = 16
IDX_DIM = 64
DSA_TOPK_MAX = 256
MOBA_BLOCK = 256
MOBA_TOPB_MAX = 3
MLA_HEADS = 16
Q_LORA = 512
KV_LORA = 512
QK_NOPE = 128
QK_ROPE = 64
V_DIM = 128
N_EXPERTS = 32
TOP_K = 4
EXPERT_DIM = 512
SWIGLU_LIMIT = 7.0
SWIGLU_ALPHA = 1.702
Q_BLOCK = 128
MOBA_Q_BLOCK = 64
N_MOD = 6
DEEPNORM_ALPHA = (2 * DEPTH) ** 0.25
DEEPNORM_BETA = (8 * DEPTH) ** -0.25
N_EVEN = (DEPTH + 1) // 2
N_ODD = DEPTH // 2
EVEN_SPLITS = (DSA_HEADS * HEAD_DIM, DSA_HEADS * HEAD_DIM, DSA_HEADS * HEAD_DIM,
               IDX_HEADS * IDX_DIM, IDX_DIM, IDX_HEADS,
               MOBA_HEADS * HEAD_DIM, MOBA_HEADS * HEAD_DIM, MOBA_HEADS * HEAD_DIM)
EVEN_IN = sum(EVEN_SPLITS)
EVEN_OUT = (DSA_HEADS + MOBA_HEADS) * HEAD_DIM
MLA_DOWN = Q_LORA + KV_LORA + QK_ROPE
NEG = -1e30

kernel_name = 'hybrid_dsa_moba_mla_moe_deepnorm_adaln'


def split_cols(a, sizes):
    offsets = [int(o) for o in np.cumsum(sizes)[:-1]]
    return jnp.split(a, offsets, axis=-1)


def layer_norm(x, g, b, eps=1e-5):
    xf = x.astype(jnp.float32)
    mu = jnp.mean(xf, axis=-1, keepdims=True)
    var = jnp.mean(jnp.square(xf - mu), axis=-1, keepdims=True)
    return ((xf - mu) * lax.rsqrt(var + eps) * g + b).astype(x.dtype)


def rms_norm(x, g, eps=1e-6):
    xf = x.astype(jnp.float32)
    return (xf * lax.rsqrt(jnp.mean(xf * xf, axis=-1, keepdims=True) + eps) * g).astype(x.dtype)


def rope_tables(seq, dim):
    inv = 1.0 / (ROPE_THETA ** (jnp.arange(0, dim, 2, dtype=jnp.float32) / dim))
    ang = jnp.arange(seq, dtype=jnp.float32)[:, None] * inv[None, :]
    return jnp.cos(ang), jnp.sin(ang)


def apply_rope(x, cos, sin):
    c = cos[None, :, None, :].astype(x.dtype)
    s = sin[None, :, None, :].astype(x.dtype)
    x1, x2 = jnp.split(x, 2, axis=-1)
    return jnp.concatenate([x1 * c - x2 * s, x1 * s + x2 * c], axis=-1)


def dsa_attention(q, k, v, q_idx, k_idx, w_idx):
    B, S, H, D = q.shape
    topk = min(DSA_TOPK_MAX, S // 4)
    scale = HEAD_DIM ** -0.5
    idx_scale = IDX_DIM ** -0.5
    w_scaled = w_idx.astype(jnp.float32) * (IDX_HEADS ** -0.5)
    key_pos = jnp.arange(S)
    gather = jax.vmap(lambda kk, ii: kk[ii])

    def block(i):
        t0 = i * Q_BLOCK
        qb = lax.dynamic_slice_in_dim(q, t0, Q_BLOCK, axis=1)
        qib = lax.dynamic_slice_in_dim(q_idx, t0, Q_BLOCK, axis=1)
        wb = lax.dynamic_slice_in_dim(w_scaled, t0, Q_BLOCK, axis=1)
        q_pos = t0 + jnp.arange(Q_BLOCK)
        causal = key_pos[None, :] <= q_pos[:, None]
        logits = jnp.einsum('bthd,bsd->bths', qib, k_idx).astype(jnp.float32) * idx_scale
        score = jnp.einsum('bth,bths->bts', wb, jax.nn.relu(logits))
        score = jnp.where(causal[None], score, -jnp.inf)
        _, sel = lax.top_k(score, topk)
        valid = sel <= q_pos[None, :, None]
        kg = gather(k, sel)
        vg = gather(v, sel)
        s = jnp.einsum('bthd,btkhd->bhtk', qb, kg).astype(jnp.float32) * scale
        s = jnp.where(valid[:, None], s, NEG)
        p = jax.nn.softmax(s, axis=-1).astype(v.dtype)
        return jnp.einsum('bhtk,btkhd->bthd', p, vg)

    out = lax.map(block, jnp.arange(S // Q_BLOCK))
    return out.transpose(1, 0, 2, 3, 4).reshape(B, S, H * D)


def moba_attention(q, k, v):
    B, S, H, D = q.shape
    nb = -(-S // MOBA_BLOCK)
    pad = nb * MOBA_BLOCK - S
    topb = min(MOBA_TOPB_MAX, nb)
    scale = HEAD_DIM ** -0.5
    padw = ((0, 0), (0, pad), (0, 0), (0, 0))
    kbh = jnp.pad(k, padw).reshape(B, nb, MOBA_BLOCK, H, D).transpose(0, 3, 1, 2, 4)
    vbh = jnp.pad(v, padw).reshape(B, nb, MOBA_BLOCK, H, D).transpose(0, 3, 1, 2, 4)
    kmean = jnp.mean(kbh, axis=3)
    qbh = q.transpose(0, 2, 1, 3)
    blk_ids = jnp.arange(nb)
    in_blk = jnp.arange(MOBA_BLOCK)
    gather = jax.vmap(jax.vmap(lambda kk, ii: kk[ii]))

    def block(i):
        t0 = i * MOBA_Q_BLOCK
        qb = lax.dynamic_slice_in_dim(qbh, t0, MOBA_Q_BLOCK, axis=2)
        q_pos = t0 + jnp.arange(MOBA_Q_BLOCK)
        own = t0 // MOBA_BLOCK
        gate = jnp.einsum('bhtd,bhnd->bhtn', qb, kmean).astype(jnp.float32)
        gate = jnp.where((blk_ids < own)[None, None, None, :], gate, -jnp.inf)
        _, sel = lax.top_k(gate, topb)
        sel_valid = sel < own
        kg = gather(kbh, sel)
        vg = gather(vbh, sel)
        s_sel = jnp.einsum('bhtd,bhtnkd->bhtnk', qb, kg).astype(jnp.float32) * scale
        s_sel = jnp.where(sel_valid[..., None], s_sel, NEG).reshape(B, H, MOBA_Q_BLOCK, topb * MOBA_BLOCK)
        k_own = lax.dynamic_slice_in_dim(kbh, own, 1, axis=2)[:, :, 0]
        v_own = lax.dynamic_slice_in_dim(vbh, own, 1, axis=2)[:, :, 0]
        s_own = jnp.einsum('bhtd,bhkd->bhtk', qb, k_own).astype(jnp.float32) * scale
        own_causal = (own * MOBA_BLOCK + in_blk)[None, :] <= q_pos[:, None]
        s_own = jnp.where(own_causal[None, None], s_own, NEG)
        p = jax.nn.softmax(jnp.concatenate([s_sel, s_own], axis=-1), axis=-1).astype(v.dtype)
        p_sel = p[..., :topb * MOBA_BLOCK].reshape(B, H, MOBA_Q_BLOCK, topb, MOBA_BLOCK)
        p_own = p[..., topb * MOBA_BLOCK:]
        return (jnp.einsum('bhtnk,bhtnkd->bhtd', p_sel, vg)
                + jnp.einsum('bhtk,bhkd->bhtd', p_own, v_own))

    out = lax.map(block, jnp.arange(S // MOBA_Q_BLOCK))
    return out.transpose(1, 0, 3, 2, 4).reshape(B, S, H * D)


def even_mixer(h, w_in, w_out, k_ln_g, k_ln_b, cos_h, sin_h, cos_i, sin_i):
    B, S, _ = h.shape
    qa, ka, va, qi, ki, wi, qb, kb, vb = split_cols(h @ w_in, EVEN_SPLITS)
    qa = apply_rope(qa.reshape(B, S, DSA_HEADS, HEAD_DIM), cos_h, sin_h)
    ka = apply_rope(ka.reshape(B, S, DSA_HEADS, HEAD_DIM), cos_h, sin_h)
    va = va.reshape(B, S, DSA_HEADS, HEAD_DIM)
    qi = apply_rope(qi.reshape(B, S, IDX_HEADS, IDX_DIM), cos_i, sin_i)
    ki = apply_rope(layer_norm(ki, k_ln_g, k_ln_b)[:, :, None, :], cos_i, sin_i)[:, :, 0]
    qb = apply_rope(qb.reshape(B, S, MOBA_HEADS, HEAD_DIM), cos_h, sin_h)
    kb = apply_rope(kb.reshape(B, S, MOBA_HEADS, HEAD_DIM), cos_h, sin_h)
    vb = vb.reshape(B, S, MOBA_HEADS, HEAD_DIM)
    out_a = dsa_attention(qa, ka, va, qi, ki, wi)
    out_b = moba_attention(qb, kb, vb)
    return jnp.concatenate([out_a, out_b], axis=-1) @ w_out


def mla_mixer(h, w_down, q_norm_g, kv_norm_g, w_q_up, w_kv_up, w_out, cos_r, sin_r):
    B, S, _ = h.shape
    cq, ckv, k_rope = split_cols(h @ w_down, (Q_LORA, KV_LORA, QK_ROPE))
    q = (rms_norm(cq, q_norm_g) @ w_q_up).reshape(B, S, MLA_HEADS, QK_NOPE + QK_ROPE)
    q_nope = q[..., :QK_NOPE]
    q_rope = apply_rope(q[..., QK_NOPE:], cos_r, sin_r)
    k_rope = apply_rope(k_rope[:, :, None, :], cos_r, sin_r)[:, :, 0]
    kv = (rms_norm(ckv, kv_norm_g) @ w_kv_up).reshape(B, S, MLA_HEADS, QK_NOPE + V_DIM)
    k_nope = kv[..., :QK_NOPE]
    v = kv[..., QK_NOPE:]
    scale = (QK_NOPE + QK_ROPE) ** -0.5
    key_pos = jnp.arange(S)

    def block(i):
        t0 = i * Q_BLOCK
        qn = lax.dynamic_slice_in_dim(q_nope, t0, Q_BLOCK, axis=1)
        qr = lax.dynamic_slice_in_dim(q_rope, t0, Q_BLOCK, axis=1)
        q_pos = t0 + jnp.arange(Q_BLOCK)
        s = (jnp.einsum('bthd,bshd->bhts', qn, k_nope)
             + jnp.einsum('bthd,bsd->bhts', qr, k_rope)).astype(jnp.float32) * scale
        s = jnp.where((key_pos[None, :] <= q_pos[:, None])[None, None], s, NEG)
        p = jax.nn.softmax(s, axis=-1).astype(v.dtype)
        return jnp.einsum('bhts,bshd->bthd', p, v)

    out = lax.map(block, jnp.arange(S // Q_BLOCK))
    return out.transpose(1, 0, 2, 3, 4).reshape(B, S, MLA_HEADS * V_DIM) @ w_out


def clamped_swiglu(gu):
    x_glu = jnp.minimum(gu[..., ::2], SWIGLU_LIMIT)
    x_lin = jnp.clip(gu[..., 1::2], -SWIGLU_LIMIT, SWIGLU_LIMIT)
    return x_glu * jax.nn.sigmoid(SWIGLU_ALPHA * x_glu) * (x_lin + 1.0)


def moe(h, w_r, b_r, w_gu, b_gu, w_dn, b_dn):
    B, S, D = h.shape
    t = h.reshape(B * S, D)
    logits = (t @ w_r + b_r).astype(jnp.float32)
    top_val, top_idx = lax.top_k(logits, TOP_K)
    gates = jax.nn.softmax(top_val, axis=-1)
    combine = jnp.einsum('tk,tke->te', gates,
                         jax.nn.one_hot(top_idx, N_EXPERTS, dtype=jnp.float32)).astype(t.dtype)
    y = jnp.zeros_like(t)
    for e in range(N_EXPERTS):
        act = clamped_swiglu(t @ w_gu[e] + b_gu[e])
        y = y + combine[:, e:e + 1] * (act @ w_dn[e] + b_dn[e])
    return y.reshape(B, S, D)


def setup_inputs(seed: int = 0) -> dict:
    key = jax.random.key(seed)
    ks = jax.random.split(key, 24)
    D = D_MODEL

    def nrm(k, shape, scale):
        return jax.random.normal(k, shape, jnp.float32) * scale

    return {
        'x': nrm(ks[0], (BATCH, SEQ, D), 1.0),
        'c': nrm(ks[1], (BATCH, D), 1.0),
        'w_mod': nrm(ks[2], (DEPTH, D, N_MOD * D), 0.2 * D ** -0.5),
        'b_mod': nrm(ks[3], (DEPTH, N_MOD * D), 0.01),
        'ln1_g': 1.0 + nrm(ks[4], (DEPTH, D), 0.01),
        'ln1_b': nrm(ks[5], (DEPTH, D), 0.01),
        'ln2_g': 1.0 + nrm(ks[6], (DEPTH, D), 0.01),
        'ln2_b': nrm(ks[7], (DEPTH, D), 0.01),
        'even_w_in': nrm(ks[8], (N_EVEN, D, EVEN_IN), D ** -0.5),
        'even_w_out': nrm(ks[9], (N_EVEN, EVEN_OUT, D), DEEPNORM_BETA * EVEN_OUT ** -0.5),
        'idx_ln_g': 1.0 + nrm(ks[10], (N_EVEN, IDX_DIM), 0.01),
        'idx_ln_b': nrm(ks[11], (N_EVEN, IDX_DIM), 0.01),
        'mla_w_down': nrm(ks[12], (N_ODD, D, MLA_DOWN), D ** -0.5),
        'mla_q_norm': 1.0 + nrm(ks[13], (N_ODD, Q_LORA), 0.01),
        'mla_kv_norm': 1.0 + nrm(ks[14], (N_ODD, KV_LORA), 0.01),
        'mla_w_q_up': nrm(ks[15], (N_ODD, Q_LORA, MLA_HEADS * (QK_NOPE + QK_ROPE)), Q_LORA ** -0.5),
        'mla_w_kv_up': nrm(ks[16], (N_ODD, KV_LORA, MLA_HEADS * (QK_NOPE + V_DIM)), KV_LORA ** -0.5),
        'mla_w_out': nrm(ks[17], (N_ODD, MLA_HEADS * V_DIM, D), DEEPNORM_BETA * (MLA_HEADS * V_DIM) ** -0.5),
        'router_w': nrm(ks[18], (DEPTH, D, N_EXPERTS), D ** -0.5),
        'router_b': nrm(ks[19], (DEPTH, N_EXPERTS), 0.01),
        'exp_w_gu': nrm(ks[20], (DEPTH, N_EXPERTS, D, 2 * EXPERT_DIM), D ** -0.5),
        'exp_b_gu': nrm(ks[21], (DEPTH, N_EXPERTS, 2 * EXPERT_DIM), 0.01),
        'exp_w_down': nrm(ks[22], (DEPTH, N_EXPERTS, EXPERT_DIM, D), DEEPNORM_BETA * EXPERT_DIM ** -0.5),
        'exp_b_down': nrm(ks[23], (DEPTH, N_EXPERTS, D), 0.01),
    }


def reference(x, c, w_mod, b_mod, ln1_g, ln1_b, ln2_g, ln2_b, even_w_in, even_w_out,
              idx_ln_g, idx_ln_b, mla_w_down, mla_q_norm, mla_kv_norm, mla_w_q_up,
              mla_w_kv_up, mla_w_out, router_w, router_b, exp_w_gu, exp_b_gu,
              exp_w_down, exp_b_down):
    S = x.shape[1]
    cos_h, sin_h = rope_tables(S, HEAD_DIM)
    cos_i, sin_i = rope_tables(S, IDX_DIM)
    cos_r, sin_r = rope_tables(S, QK_ROPE)
    c_act = jax.nn.silu(c)
    for l in range(DEPTH):
        mod = c_act @ w_mod[l] + b_mod[l]
        sh1, sc1, g1, sh2, sc2, g2 = [m[:, None, :] for m in jnp.split(mod, N_MOD, axis=-1)]
        h = x * (1.0 + sc1) + sh1
        j = l // 2
        if l % 2 == 0:
            y = even_mixer(h, even_w_in[j], even_w_out[j], idx_ln_g[j], idx_ln_b[j],
                           cos_h, sin_h, cos_i, sin_i)
        else:
            y = mla_mixer(h, mla_w_down[j], mla_q_norm[j], mla_kv_norm[j], mla_w_q_up[j],
                          mla_w_kv_up[j], mla_w_out[j], cos_r, sin_r)
        x = layer_norm(DEEPNORM_ALPHA * x + (1.0 + g1) * y, ln1_g[l], ln1_b[l])
        h = x * (1.0 + sc2) + sh2
        y = moe(h, router_w[l], router_b[l], exp_w_gu[l], exp_b_gu[l], exp_w_down[l], exp_b_down[l])
        x = layer_norm(DEEPNORM_ALPHA * x + (1.0 + g2) * y, ln2_g[l], ln2_b[l])
    return x
```

```python
import os
import numpy as np
import ml_dtypes
KSTOP = os.environ.get('KSTOP', '')
import concourse.bass as bass
import concourse.mybir as mybir
from concourse.bass_utils import run_bass_kernel_spmd

F32 = mybir.dt.float32
BF16 = mybir.dt.bfloat16
AF = mybir.ActivationFunctionType
ALU = mybir.AluOpType
AX = mybir.AxisListType

NEG = -1e30
NC_ = 2


class Buf:
    __slots__ = ("ws", "rs", "prs", "name")

    def __init__(self, name=""):
        self.ws = []
        self.rs = []
        self.prs = []
        self.name = name


class Op:
    __slots__ = ("stream", "fn", "deps", "sig", "count", "asem", "aval", "ainc", "prev_async", "seen")

    def __init__(self, stream, fn):
        self.stream = stream
        self.fn = fn
        self.deps = []
        self.sig = False
        self.count = 0
        self.asem = None
        self.aval = 0
        self.ainc = 0
        self.prev_async = None


class Sched:
    STREAMS = ("pe", "act", "dve", "pool", "sp")

    def __init__(self, nc, sems):
        self.nc = nc
        self.ops = {s: [] for s in self.STREAMS}
        self.sem = {s: sems.pop() for s in ("pe", "act", "dve", "pool")}
        npool = {"sp": 16, "pool": 8, "act": 4}
        self.dpool = {s: [sems.pop() for _ in range(n)] for s, n in npool.items()}
        self.dnext = {s: 0 for s in npool}
        self.dlast = {s: [None] * n for s, n in npool.items()}
        self.free_sems = sems
        self.barrier_ops = {s: [] for s in self.STREAMS}
        self.all_async = []

    def _add(self, stream, meth, args, kw, async_inc=0, dedicated_sem=None):
        r = kw.pop("r", ())
        w = kw.pop("w", ())

        def fn(e, meth=meth, args=args, kw=kw):
            return getattr(e, meth)(*args, **kw)
        op = Op(stream, fn)
        deps = []
        for b in r:
            deps.extend(b.ws)
        for b in w:
            deps.extend(b.ws)
            deps.extend(b.rs)
        deps.extend(self.barrier_ops[stream])
        self.barrier_ops[stream] = []
        is_async = async_inc > 0
        op.seen = set()
        for d in deps:
            self._dep(op, d)
        if is_async:
            if dedicated_sem is not None:
                op.asem = dedicated_sem
                op.aval = async_inc
                op.ainc = async_inc
            else:
                k = self.dnext[stream]
                self.dnext[stream] = (k + 1) % len(self.dpool[stream])
                prev = self.dlast[stream][k]
                op.asem = self.dpool[stream][k]
                op.ainc = async_inc
                op.aval = (prev.aval if prev is not None else 0) + async_inc
                op.prev_async = prev
                self.dlast[stream][k] = op
            self.all_async.append(op)
        for b in r:
            b.rs.append(op)
        for b in w:
            b.prs = b.rs
            b.ws = [op]
            b.rs = []
        self.ops[stream].append(op)
        return op

    def _dep(self, op, d):
        if id(d) in op.seen or d is op:
            return
        op.seen.add(id(d))
        if d.asem is None and d.stream == op.stream and op.stream == "pe":
            return
        op.deps.append(d)
        if d.asem is None:
            d.sig = True

    def pe(self, meth, *args, **kw):
        return self._add("pe", meth, args, kw)

    def act(self, meth, *args, **kw):
        return self._add("act", meth, args, kw)

    def dve(self, meth, *args, **kw):
        return self._add("dve", meth, args, kw)

    def pool(self, meth, *args, **kw):
        return self._add("pool", meth, args, kw)

    def dma(self, stream, meth, *args, **kw):
        return self._add(stream, meth, args, kw, async_inc=16)

    def collective(self, meth, *args, **kw):
        return self._add("pool", meth, args, kw, async_inc=1, dedicated_sem=self.free_sems.pop())

    def multi_w(self, bufs, op):
        for b in bufs:
            for d in b.prs:
                self._dep(op, d)
            for d in b.rs:
                self._dep(op, d)
            b.ws.append(op)

    def barrier(self):
        lasts = []
        for s in self.STREAMS:
            if self.ops[s]:
                lasts.append(self.ops[s][-1])
        pend = [o for o in self.all_async]
        self.all_async = []
        for s in self.STREAMS:
            self.barrier_ops[s] = list(lasts) + pend

    def emit(self, block):
        nc = self.nc
        for s in ("pe", "act", "dve", "pool"):
            c = 0
            for op in self.ops[s]:
                if op.sig and op.asem is None:
                    c += 1
                    op.count = c
        engs = {"pe": "tensor", "act": "scalar", "dve": "vector", "pool": "gpsimd", "sp": "sync"}

        def run(stream, eng):
            known = {}

            def wait(sem, val):
                if val <= 0:
                    return
                k = id(sem)
                if known.get(k, 0) >= val:
                    return
                known[k] = val
                eng.wait_ge(sem, val)

            for op in self.ops[stream]:
                need = {}
                for d in op.deps:
                    if d.asem is not None:
                        sem, val = d.asem, d.aval
                    else:
                        sem, val = self.sem[d.stream], d.count
                    k = id(sem)
                    if k not in need or need[k][1] < val:
                        need[k] = (sem, val)
                if op.prev_async is not None:
                    sem, val = op.prev_async.asem, op.prev_async.aval
                    k = id(sem)
                    if k not in need or need[k][1] < val:
                        need[k] = (sem, val)
                for sem, val in need.values():
                    wait(sem, val)
                inst = op.fn(eng)
                if op.asem is not None:
                    inst.then_inc(op.asem, op.ainc)
                elif op.sig:
                    inst.then_inc(self.sem[stream], 1)
            for k, lst in enumerate(self.dlast.get(stream, [])):
                if lst is not None:
                    wait(lst.asem, lst.aval)

        @block.tensor
        def _(e):
            run("pe", e)

        @block.scalar
        def _(e):
            run("act", e)

        @block.vector
        def _(e):
            run("dve", e)

        @block.gpsimd
        def _(e):
            run("pool", e)

        @block.sync
        def _(e):
            run("sp", e)


class Cfg:
    def __init__(self, S, D, DEPTH, NE, TG=2048, EG=512, parity=0, alpha=None):
        self.S, self.D, self.DEPTH, self.NE = S, D, DEPTH, NE
        self.parity = parity
        self.KC = D // 128
        self.NT = S // 128
        self.TG = min(TG, S)
        self.NTG = self.TG // 128
        self.NG = S // self.TG
        self.EG = min(EG, S)
        self.kinds = [(l + parity) % 2 for l in range(DEPTH)]
        self.N_EVEN = self.kinds.count(0)
        self.N_ODD = self.kinds.count(1)
        self.jl = [self.kinds[:l].count(self.kinds[l]) for l in range(DEPTH)]
        self.alpha = float((2 * DEPTH) ** 0.25) if alpha is None else float(alpha)
        self.NB = S // 256
        self.topk = min(256, S // 4)
        self.topb = min(3, self.NB)
        self.EO = dict(qa=0, ka=1024, va=2048, qi=3072, ki=4096, wi=4160, qb=4176, kb=5200, vb=6224)
        self.EIN = 7248


def build_program(cfg, stages=None, dbg=()):
    c = cfg
    S, D, KC, NT, NE, DEPTH = c.S, c.D, c.KC, c.NT, c.NE, c.DEPTH
    TG, NTG, NG, NB = c.TG, c.NTG, c.NG, c.NB
    NEV, NOD = max(c.N_EVEN, 1), max(c.N_ODD, 1)
    HE, HO = c.N_EVEN > 0, c.N_ODD > 0
    nc = bass.Bass("TRN2", target_bir_lowering=False)

    def din(name, shape, dt=F32):
        return nc.dram_tensor(name, list(shape), dt, kind="ExternalInput").ap()

    scr = {}

    def dscr(name, shape, dt=F32):
        ap = nc.dram_tensor(name, list(shape), dt, kind="Internal").ap()
        scr[name] = (ap, list(shape), dt)
        return ap

    x_in = din("x", [S, D])
    cT_in = din("cT", [128, KC])
    wmod_l = [din(f"w_mod{l}", [D, 6 * D]) for l in range(DEPTH)]
    bmod_in = din("b_mod", [DEPTH, 6 * D])
    ln_in = din("lnp", [DEPTH * 4, D])
    ewin_in = din("even_w_in", [NEV * D if HE else 1, c.EIN])
    ewout_in = din("even_w_out", [NEV * 2048 if HE else 1, D])
    idxln_in = din("idxln", [NEV * 2, 64])
    mdown_in = din("mla_w_down", [NOD * D if HO else 1, 1088])
    mlan_in = din("mlan", [NOD * 2, 512])
    mqup_in = din("mla_w_q_up", [NOD * 512 if HO else 1, 3072])
    mkvup_in = din("mla_w_kv_up", [NOD * 512 if HO else 1, 4096])
    mwout_in = din("mla_w_out", [NOD * 2048 if HO else 1, D])
    rw_in = din("rw", [DEPTH * D, NE])
    rb_in = din("rb", [DEPTH, NE])
    wgu_l = [din(f"wgu{l}", [NE * 512, KC * 256]) for l in range(DEPTH)]
    bgu_in = din("bgu", [DEPTH * NE * 8, 128])
    wdn_l = [din(f"wdn{l}", [NE * 128, 4 * D]) for l in range(DEPTH)]
    bdn_in = din("bdn", [DEPTH * NE, D])
    ident_in = din("ident", [128, 128])
    sel_in = din("sel", [NE, NE * 128], BF16)
    rope_in = din("rope", [NT * 128, 192])
    cmask_in = din("cmask", [128, 4 * 512])
    mmoba_in = din("mmoba", [128, 4 * 512])
    gmo_in = din("gmo", [NT, 2 * NB])
    y_out = nc.dram_tensor("y", [S, D], F32, kind="ExternalOutput").ap()

    modv = dscr("modv", [DEPTH * 6, D])
    ewin_b = dscr("ewin_b", [NEV * D if HE else 1, c.EIN], BF16)
    ewout_b = dscr("ewout_b", [NEV * 2048 if HE else 1, D], BF16)
    mdown_b = dscr("mdown_b", [NOD * D if HO else 1, 1088], BF16)
    mqup_b = dscr("mqup_b", [NOD * 512 if HO else 1, 3072], BF16)
    mkvup_b = dscr("mkvup_b", [NOD * 512 if HO else 1, 4096], BF16)
    mwout_b = dscr("mwout_b", [NOD * 2048 if HO else 1, D], BF16)
    wgu_bl = [dscr(f"wgu_b{l}", [NE * 512, KC * 256], BF16) for l in range(DEPTH)]
    wdn_bl = [dscr(f"wdn_b{l}", [NE * 128, 4 * D], BF16) for l in range(DEPTH)]
    xmid = dscr("xmid", [S, D])
    xres = dscr("xres", [S, D])
    ao_d = dscr("ao", [S, 2048], BF16)
    qT_d = dscr("qT", [24 * 128, S], BF16)
    KT_d = dscr("KT", [17 * 128, S], BF16)
    V_d = dscr("V", [16 * 128, NT * 128], BF16)
    wi_d = dscr("wi_d", [S, 32])

    import contextlib
    es = contextlib.ExitStack()
    psb = [es.enter_context(nc.psum_tensor(f"ps{k}", [128, 512], F32)) for k in range(8)]
    sems = [es.enter_context(nc.semaphore(f"s{k}")) for k in range(40)]
    block = es.enter_context(nc.Block())
    Sx = Sched(nc, sems)
    PB = [Buf(f"psum{k}") for k in range(8)]

    stacks = [contextlib.ExitStack()]
    tcount = [0]

    def salloc(cols, dt=F32):
        tcount[0] += 1
        t = stacks[-1].enter_context(nc.sbuf_tensor(f"t{tcount[0]}", [128, cols], dt))
        return t[:, :]

    def smark():
        stacks.append(contextlib.ExitStack())
        return len(stacks) - 1

    def srelease(m):
        while len(stacks) > m:
            stacks.pop().close()

    def psf(k):
        return psb[k][:, :]

    def psh(k):
        return psb[k][:, :].bitcast(BF16)

    def W(bufs, op):
        Sx.multi_w(bufs, op)

    ident_f = salloc(128)
    ident_b = salloc(128, BF16)
    cmask = salloc(4 * 512)
    mmoba = salloc(4 * 512)
    eps5 = salloc(2)
    B_const = Buf("const")
    Sx.dma("sp", "dma_start", out=ident_f, in_=ident_in, w=[B_const])
    W([B_const], Sx.dma("sp", "dma_start", out=cmask, in_=cmask_in))
    W([B_const], Sx.dma("sp", "dma_start", out=mmoba, in_=mmoba_in))
    B_identb = Buf("identb")
    Sx.dve("tensor_copy", out=ident_b, in_=ident_f, r=[B_const], w=[B_identb])
    B_eps = Buf()
    Sx.dve("memset", eps5[:, 0:1], 1e-5, w=[B_eps])
    W([B_eps], Sx.dve("memset", eps5[:, 1:2], 1e-6))
    cmask3 = cmask.rearrange("p (r k) -> p r k", r=4)
    mmoba3 = mmoba.rearrange("p (r k) -> p r k", r=4)

    def run_stage(name):
        return stages is None or name in stages

    B_w = {}

    def cast_tensor(key, src, dst, r0, nrows, d0=None):
        b = Buf(key)
        B_w[key] = b
        ncols = src.shape[1]
        if d0 is None:
            d0 = r0
        step = max(128, (1 << 20) // ncols // 128 * 128)
        for a in range(0, nrows, step):
            e = min(nrows, a + step)
            W([b], Sx.dma("pool", "dma_start", out=dst[d0 + a:d0 + e, :], in_=src[r0 + a:r0 + e, :]))

    if run_stage("W"):
        for l in range(DEPTH):
            j = c.jl[l]
            if c.kinds[l] == 0:
                cast_tensor(("win", l), ewin_in, ewin_b, j * D, D)
                cast_tensor(("wout", l), ewout_in, ewout_b, j * 2048, 2048)
            else:
                cast_tensor(("wdown", l), mdown_in, mdown_b, j * D, D)
                cast_tensor(("wqup", l), mqup_in, mqup_b, j * 512, 512)
                cast_tensor(("wkvup", l), mkvup_in, mkvup_b, j * 512, 512)
                cast_tensor(("wout", l), mwout_in, mwout_b, j * 2048, 2048)
            cast_tensor(("wgu", l), wgu_l[l], wgu_bl[l], 0, NE * 512, d0=0)
            cast_tensor(("wdn", l), wdn_l[l], wdn_bl[l], 0, NE * 128, d0=0)

    def Bw(key):
        return [B_w[key]] if key in B_w else []

    B_mod = Buf("modv")
    if run_stage("M"):
        m0 = smark()
        cT = salloc(KC)
        scT = salloc(KC)
        wm = [salloc(KC * 512) for _ in range(2)]
        bm = [salloc(512) for _ in range(2)]
        mrow = [salloc(512) for _ in range(2)]
        B_cT, B_scT = Buf(), Buf()
        B_wm, B_bm, B_mrow = [Buf(), Buf()], [Buf(), Buf()], [Buf(), Buf()]
        Sx.dma("sp", "dma_start", out=cT, in_=cT_in, w=[B_cT])
        Sx.act("activation", out=scT, in_=cT, func=AF.Silu, r=[B_cT], w=[B_scT])
        cnt = 0
        for l in range(DEPTH):
            for n0 in range(0, 6 * D, 512):
                k = cnt % 2
                cnt += 1
                wm3 = wm[k].rearrange("p (k n) -> p k n", n=512)
                Sx.dma("sp", "dma_start", out=wm3, in_=wmod_l[l][:, n0:n0 + 512].rearrange("(k p) n -> p k n", p=128), w=[B_wm[k]])
                Sx.dma("sp", "dma_start", out=bm[k][0:1, :], in_=bmod_in[l:l + 1, n0:n0 + 512], w=[B_bm[k]])
                for kc in range(KC):
                    Sx.pe("matmul", psf(k)[0:1, :], lhsT=scT[:, kc:kc + 1], rhs=wm3[:, kc, :], start=(kc == 0), stop=(kc == KC - 1),
                          r=[B_scT, B_wm[k]], w=[PB[k]])
                Sx.dve("tensor_tensor", out=mrow[k][0:1, :], in0=psf(k)[0:1, :], in1=bm[k][0:1, :], op=ALU.add, r=[PB[k], B_bm[k]], w=[B_mrow[k]])
                m, d0 = n0 // D, n0 % D
                W([B_mod], Sx.dma("sp", "dma_start", out=modv[l * 6 + m:l * 6 + m + 1, d0:d0 + 512], in_=mrow[k][0:1, :], r=[B_mrow[k]]))
        Sx.barrier()
        srelease(m0)

    def load_bvec(dst, src_row, B_dst, r=(), add1=False):
        n = src_row.shape[1]
        Sx.dma("sp", "dma_start", out=dst, in_=src_row.to_broadcast([128, n]), r=list(r), w=[B_dst])
        if add1:
            Sx.dve("tensor_scalar", out=dst, in0=dst, scalar1=1.0, scalar2=None, op0=ALU.add, r=[B_dst], w=[B_dst])

    def build_hT(l, which, x_src, t0, ntile, hT3, B_hT, hook=None, B_src=None):
        m0 = smark()
        scp = salloc(D)
        sh = salloc(D)
        B_scp, B_sh = Buf(), Buf()
        load_bvec(sh, modv[l * 6 + 3 * which:l * 6 + 3 * which + 1, :], B_sh, r=[B_mod])
        load_bvec(scp, modv[l * 6 + 3 * which + 1:l * 6 + 3 * which + 2, :], B_scp, r=[B_mod], add1=True)
        xt = [salloc(D) for _ in range(2)]
        hb = [salloc(D, BF16) for _ in range(2)]
        B_xt = [Buf(), Buf()]
        B_hb = [Buf(), Buf()]
        for i in range(ntile):
            k = i % 2
            Sx.dma("sp", "dma_start", out=xt[k], in_=x_src[t0 + i * 128:t0 + (i + 1) * 128, :], r=[B_src if B_src is not None else B_x], w=[B_xt[k]])
            Sx.dve("tensor_tensor", out=xt[k], in0=xt[k], in1=scp, op=ALU.mult, r=[B_xt[k], B_scp], w=[B_xt[k]])
            Sx.dve("tensor_tensor", out=xt[k], in0=xt[k], in1=sh, op=ALU.add, r=[B_xt[k], B_sh], w=[B_xt[k]])
            Sx.act("activation", out=hb[k], in_=xt[k], func=AF.Copy, r=[B_xt[k]], w=[B_hb[k]])
            for g in range(KC // 8):
                pk = 6 + (g % 2)
                for q in range(8):
                    kc = g * 8 + q
                    Sx.pe("transpose", out=psh(pk)[:, q * 128:(q + 1) * 128], in_=hb[k][:, kc * 128:(kc + 1) * 128], identity=ident_b,
                          r=[B_hb[k], B_identb], w=[PB[pk]])
                W([B_hT], Sx.act("activation", out=hT3[:, g * 8:(g + 1) * 8, i * 128:(i + 1) * 128],
                                 in_=psh(pk).rearrange("p (q t) -> p q t", q=8), func=AF.Copy, r=[PB[pk]]))
            if hook is not None:
                hook(i, xt[k], B_xt[k])
        Sx.barrier()
        srelease(m0)

    def rope_ops(src3, dst3, cosb, sinb, half, ta, tbb, B_src, B_ta, B_tb, B_dst):
        x1, x2 = src3[:, :, 0:half], src3[:, :, half:2 * half]
        Sx.dve("tensor_tensor", out=ta, in0=x1, in1=cosb, op=ALU.mult, r=[B_src, B_rope], w=[B_ta])
        Sx.dve("tensor_tensor", out=tbb, in0=x2, in1=sinb, op=ALU.mult, r=[B_src, B_rope], w=[B_tb])
        Sx.dve("tensor_tensor", out=dst3[:, :, 0:half], in0=ta, in1=tbb, op=ALU.subtract, r=[B_ta, B_tb], w=[B_dst])
        Sx.dve("tensor_tensor", out=ta, in0=x2, in1=cosb, op=ALU.mult, r=[B_src, B_rope], w=[B_ta])
        Sx.dve("tensor_tensor", out=tbb, in0=x1, in1=sinb, op=ALU.mult, r=[B_src, B_rope], w=[B_tb])
        W([B_dst], Sx.dve("tensor_tensor", out=dst3[:, :, half:2 * half], in0=ta, in1=tbb, op=ALU.add, r=[B_ta, B_tb]))

    def ln_inplace(t, n, B_t, st, B_st, eps_col):
        nch = max(1, n // 512)
        w_ = n // nch
        for q in range(nch):
            op = Sx.dve("bn_stats", out=st[:, q * 6:(q + 1) * 6], in_=t[:, q * w_:(q + 1) * w_], r=[B_t], w=[B_st] if q == 0 else [])
            if q:
                W([B_st], op)
        mv = st[:, nch * 6:nch * 6 + 4]
        Sx.dve("bn_aggr", out=mv[:, 0:2], in_=st[:, 0:nch * 6], r=[B_st], w=[B_st])
        Sx.act("activation", out=mv[:, 2:3], in_=mv[:, 1:2], func=AF.Sqrt, bias=eps5[:, eps_col:eps_col + 1], scale=1.0, r=[B_st, B_eps], w=[B_st])
        Sx.dve("reciprocal", out=mv[:, 3:4], in_=mv[:, 2:3], r=[B_st], w=[B_st])
        Sx.dve("tensor_scalar", out=t, in0=t, scalar1=mv[:, 0:1], scalar2=mv[:, 3:4], op0=ALU.subtract, op1=ALU.mult, r=[B_t, B_st], w=[B_t])

    def residual_ln(l, which, x_old, B_xold, x_new, B_xnew, t0, ntile, y_fn, prm):
        gp, lng, lnb, B_prm, xo, B_xo, yb, B_yb, st, B_st = prm
        for i in range(ntile):
            k = i % 2
            rows = slice(t0 + i * 128, t0 + (i + 1) * 128)
            Sx.dma("sp", "dma_start", out=xo[k], in_=x_old[rows, :], r=[B_xold], w=[B_xo[k]])
            y_fn(i, yb[k], B_yb[k])
            Sx.dve("tensor_tensor", out=yb[k], in0=yb[k], in1=gp, op=ALU.mult, r=[B_yb[k], B_prm], w=[B_yb[k]])
            Sx.dve("scalar_tensor_tensor", out=yb[k], in0=xo[k], scalar=c.alpha, in1=yb[k], op0=ALU.mult, op1=ALU.add, r=[B_xo[k], B_yb[k]], w=[B_yb[k]])
            ln_inplace(yb[k], D, B_yb[k], st, B_st, 0)
            Sx.dve("tensor_tensor", out=yb[k], in0=yb[k], in1=lng, op=ALU.mult, r=[B_yb[k], B_prm], w=[B_yb[k]])
            Sx.dve("tensor_tensor", out=yb[k], in0=yb[k], in1=lnb, op=ALU.add, r=[B_yb[k], B_prm], w=[B_yb[k]])
            W([B_xnew], Sx.dma("pool", "dma_start", out=x_new[rows, :], in_=yb[k], r=[B_yb[k]]))

    def residual_prm(l, which):
        gp, lng, lnb = salloc(D), salloc(D), salloc(D)
        B_prm = Buf()
        Sx.dma("sp", "dma_start", out=gp, in_=modv[l * 6 + 3 * which + 2:l * 6 + 3 * which + 3, :].to_broadcast([128, D]), r=[B_mod], w=[B_prm])
        Sx.dve("tensor_scalar", out=gp, in0=gp, scalar1=1.0, scalar2=None, op0=ALU.add, r=[B_prm], w=[B_prm])
        W([B_prm], Sx.dma("sp", "dma_start", out=lng, in_=ln_in[l * 4 + 2 * which:l * 4 + 2 * which + 1, :].to_broadcast([128, D])))
        W([B_prm], Sx.dma("sp", "dma_start", out=lnb, in_=ln_in[l * 4 + 2 * which + 1:l * 4 + 2 * which + 2, :].to_broadcast([128, D])))
        xo = [salloc(D) for _ in range(2)]
        yb = [salloc(D) for _ in range(2)]
        st = salloc((D // 512) * 6 + 8)
        return (gp, lng, lnb, B_prm, xo, [Buf(), Buf()], yb, [Buf(), Buf()], st, Buf())

    class ACtx:
        pass

    def attn_alloc():
        a = ACtx()
        a.s = salloc(S)
        a.B_s = Buf()
        a.p = salloc(S, BF16)
        a.B_p = Buf()
        a.pT = [salloc(1024, BF16) for _ in range(2)]
        a.B_pT = [Buf(), Buf()]
        a.sm = [salloc(8) for _ in range(2)]
        a.B_sm = [Buf(), Buf()]
        a.cnt = 0
        a.sc = 0
        return a

    def attn_core(a, i, score_parts, V3, B_V, scale, mode, out_ap, B_out, bias=None, B_bias=None, bb=None, B_bb=None):
        nch = i // 4 + 1
        r = i % 4
        nk = 128 * (i + 1)
        k = a.cnt % 2
        a.cnt += 1
        sm, B_sm = a.sm[k], a.B_sm[k]
        for m in range(nch):
            n = 512 if m < nch - 1 else 128 * (r + 1)
            pk = a.sc % 2
            a.sc += 1
            np_ = len(score_parts)
            for pi, (lhsT, rhs_fn, bufs) in enumerate(score_parts):
                Sx.pe("matmul", psf(pk)[:, 0:n], lhsT=lhsT, rhs=rhs_fn(m * 512, n), start=(pi == 0), stop=(pi == np_ - 1), r=bufs, w=[PB[pk]])
            dst = a.s[:, m * 512:m * 512 + n]
            lastc = (m == nch - 1)
            st_ = {"first": (m == 0)}

            def ev(stream, meth, st_=st_, **kw):
                rr = kw.pop("r")
                if st_["first"]:
                    st_["first"] = False
                    getattr(Sx, stream)(meth, r=rr, w=[a.B_s], **kw)
                else:
                    W([a.B_s], getattr(Sx, stream)(meth, r=rr, **kw))

            if mode == "dsa":
                ev("dve", "scalar_tensor_tensor", out=dst, in0=psf(pk)[:, 0:n], scalar=scale, in1=bias[:, m * 512:m * 512 + n], op0=ALU.mult, op1=ALU.add,
                   r=[PB[pk], B_bias])
            elif mode == "moba":
                if n % 256 == 0:
                    nb2 = n // 256
                    in1 = bb[:, 2 * m:2 * m + nb2].unsqueeze(2).to_broadcast([128, nb2, 256])
                    ev("dve", "scalar_tensor_tensor", out=dst.rearrange("p (b k) -> p b k", k=256), in0=psf(pk)[:, 0:n].rearrange("p (b k) -> p b k", k=256),
                       scalar=scale, in1=in1, op0=ALU.mult, op1=ALU.add, r=[PB[pk], B_bb])
                else:
                    for tt in range(n // 128):
                        in1 = bb[:, 2 * m + tt // 2:2 * m + tt // 2 + 1].to_broadcast([128, 128])
                        ev("dve", "scalar_tensor_tensor", out=dst[:, tt * 128:(tt + 1) * 128], in0=psf(pk)[:, tt * 128:(tt + 1) * 128],
                           scalar=scale, in1=in1, op0=ALU.mult, op1=ALU.add, r=[PB[pk], B_bb])
                if lastc:
                    ev("dve", "tensor_tensor", out=dst, in0=dst, in1=mmoba3[:, r, 0:n], op=ALU.add, r=[a.B_s, B_const])
            else:
                if not lastc:
                    ev("act", "activation", out=dst, in_=psf(pk)[:, 0:n], func=AF.Copy, scale=scale, r=[PB[pk]])
                else:
                    ev("dve", "scalar_tensor_tensor", out=dst, in0=psf(pk)[:, 0:n], scalar=scale, in1=cmask3[:, r, 0:n], op0=ALU.mult, op1=ALU.add,
                       r=[PB[pk], B_const])
        Sx.dve("reduce_max", out=sm[:, 0:1], in_=a.s[:, 0:nk], axis=AX.X, r=[a.B_s], w=[B_sm])
        Sx.dve("tensor_scalar", out=sm[:, 1:2], in0=sm[:, 0:1], scalar1=-1.0, scalar2=None, op0=ALU.mult, r=[B_sm], w=[B_sm])
        Sx.act("activation", out=a.p[:, 0:nk], in_=a.s[:, 0:nk], func=AF.Exp, bias=sm[:, 1:2], scale=1.0, accum_out=sm[:, 2:3], r=[a.B_s, B_sm], w=[a.B_p, B_sm])
        nkt = i + 1
        po = 4 + (a.cnt % 2)
        for g0 in range(0, nkt, 8):
            g1 = min(nkt, g0 + 8)
            tk = 2 + ((g0 // 8) % 2)
            pk2 = (g0 // 8) % 2
            for kt in range(g0, g1):
                Sx.pe("transpose", out=psh(tk)[:, (kt - g0) * 128:(kt - g0 + 1) * 128], in_=a.p[:, kt * 128:(kt + 1) * 128], identity=ident_b,
                      r=[a.B_p, B_identb], w=[PB[tk]])
            Sx.act("activation", out=a.pT[pk2][:, 0:(g1 - g0) * 128], in_=psh(tk)[:, 0:(g1 - g0) * 128], func=AF.Copy, r=[PB[tk]], w=[a.B_pT[pk2]])
            for kt in range(g0, g1):
                Sx.pe("matmul", psf(po)[:, 0:128], lhsT=a.pT[pk2][:, (kt - g0) * 128:(kt - g0 + 1) * 128], rhs=V3[:, kt, :],
                      start=(kt == 0), stop=(kt == nkt - 1), r=[a.B_pT[pk2], B_V], w=[PB[po]])
        Sx.dve("reciprocal", out=sm[:, 3:4], in_=sm[:, 2:3], r=[B_sm], w=[B_sm])
        W([B_out], Sx.dve("tensor_scalar", out=out_ap, in0=psf(po)[:, 0:128], scalar1=sm[:, 3:4], scalar2=None, op0=ALU.mult, r=[PB[po], B_sm]))

    x_cur = x_in
    B_x = Buf("x")
    B_rope = Buf("rope")
    for l in range(DEPTH):
        jl = c.jl[l]
        even = (c.kinds[l] == 0)
        last = (l == DEPTH - 1)
        if not run_stage(f"L{l}"):
            continue
        B_q, B_KT, B_V, B_wi, B_ao, B_xmid, B_xnew = Buf(), Buf(), Buf(), Buf(), Buf(), Buf(), Buf()
        qT3 = qT_d.rearrange("(h p) t -> h p t", p=128)
        KT3 = KT_d.rearrange("(h p) t -> h p t", p=128)
        V4 = V_d.rearrange("(h p) (i d) -> h p i d", p=128, d=128)
        for g in range(NG):
            t0 = g * TG
            mL = smark()
            hT = salloc(KC * TG, BF16)
            hT3 = hT.rearrange("p (k t) -> p k t", k=KC)
            B_hT = Buf()
            build_hT(l, 0, x_cur, t0, NTG, hT3, B_hT)
            ropeg = salloc(NTG * 192)
            rope3 = ropeg.rearrange("p (t k) -> p t k", k=192)
            Sx.dma("sp", "dma_start", out=rope3, in_=rope_in[t0:t0 + TG, :].rearrange("(t p) k -> p t k", p=128), w=[B_rope])
            cs = [salloc(256) for _ in range(2)]
            B_cs = [Buf(), Buf()]
            ob = [salloc(512, BF16) for _ in range(2)]
            B_ob = [Buf(), Buf()]
            tb = [salloc(512, BF16) for _ in range(2)]
            B_tb = [Buf(), Buf()]
            wch = [salloc(KC * 512, BF16) for _ in range(2)]
            B_wch = [Buf(), Buf()]
            cnt = 0

            def transpose_out(src_ob, B_src, k2, dst3d, h0, nh4, tglob, B_dst):
                tk = 4 + k2
                for q in range(nh4):
                    Sx.pe("transpose", out=psh(tk)[:, q * 128:(q + 1) * 128], in_=src_ob[:, q * 128:(q + 1) * 128], identity=ident_b,
                          r=[B_src, B_identb], w=[PB[tk]])
                Sx.act("activation", out=tb[k2][:, 0:nh4 * 128], in_=psh(tk)[:, 0:nh4 * 128], func=AF.Copy, r=[PB[tk]], w=[B_tb[k2]])
                W([B_dst], Sx.dma("pool", "dma_start", out=dst3d[h0:h0 + nh4, :, tglob:tglob + 128].rearrange("h p t -> p h t"),
                                  in_=tb[k2][:, 0:nh4 * 128].rearrange("p (h t) -> p h t", h=nh4), r=[B_tb[k2]]))

            if even:
                EO = c.EO
                lng64, lnb64 = salloc(64), salloc(64)
                B_l64 = Buf()
                Sx.dma("sp", "dma_start", out=lng64, in_=idxln_in[jl * 2:jl * 2 + 1, :].to_broadcast([128, 64]), w=[B_l64])
                W([B_l64], Sx.dma("sp", "dma_start", out=lnb64, in_=idxln_in[jl * 2 + 1:jl * 2 + 2, :].to_broadcast([128, 64])))
                kit = salloc(128)
                st8 = salloc(16)
                B_kit, B_st8 = Buf(), Buf()
                wi_t = salloc(32)
                B_wit = Buf()
                chunks = []
                for hh in range(2):
                    chunks.append((EO["qa"] + hh * 512, 512, "rope128", ("q", 0 + hh * 4)))
                for hh in range(2):
                    chunks.append((EO["ka"] + hh * 512, 512, "rope128", ("k", 0 + hh * 4)))
                for hh in range(2):
                    chunks.append((EO["va"] + hh * 512, 512, "v", (0, hh * 4)))
                for hh in range(2):
                    chunks.append((EO["qi"] + hh * 512, 512, "rope64", ("q", 16 + hh * 4)))
                chunks.append((EO["ki"], 80, "kiwi", None))
                for hh in range(2):
                    chunks.append((EO["qb"] + hh * 512, 512, "rope128", ("q", 8 + hh * 4)))
                for hh in range(2):
                    chunks.append((EO["kb"] + hh * 512, 512, "rope128", ("k", 8 + hh * 4)))
                for hh in range(2):
                    chunks.append((EO["vb"] + hh * 512, 512, "v", (1, hh * 4)))
                wsrc = ewin_b[jl * D:(jl + 1) * D, :]
                for ci, (c0, ncol, kind, dest) in enumerate(chunks):
                    wk = ci % 2
                    w3 = wch[wk].rearrange("p (k n) -> p k n", k=KC)
                    Sx.dma("sp", "dma_start", out=w3[:, :, 0:ncol], in_=wsrc[:, c0:c0 + ncol].rearrange("(k p) n -> p k n", p=128),
                           r=Bw(("win", l)), w=[B_wch[wk]])
                    for i in range(NTG):
                        pk = cnt % 2
                        k2 = cnt % 2
                        cnt += 1
                        tg = t0 + i * 128
                        for kc in range(KC):
                            Sx.pe("matmul", psf(pk)[:, 0:ncol], lhsT=hT3[:, kc, i * 128:(i + 1) * 128], rhs=w3[:, kc, 0:ncol],
                                  start=(kc == 0), stop=(kc == KC - 1), r=[B_hT, B_wch[wk]], w=[PB[pk]])
                        if kind in ("rope128", "rope64"):
                            half = 64 if kind == "rope128" else 32
                            nh = 512 // (2 * half)
                            cosb = (rope3[:, i, 0:64] if half == 64 else rope3[:, i, 128:160]).unsqueeze(1).to_broadcast([128, nh, half])
                            sinb = (rope3[:, i, 64:128] if half == 64 else rope3[:, i, 160:192]).unsqueeze(1).to_broadcast([128, nh, half])
                            p3 = psf(pk).rearrange("p (h d) -> p h d", h=nh)
                            ta = cs[0].rearrange("p (h d) -> p h d", h=nh)
                            tbb = cs[1].rearrange("p (h d) -> p h d", h=nh)
                            o3 = ob[k2].rearrange("p (h d) -> p h d", h=nh)
                            rope_ops(p3, o3, cosb, sinb, half, ta, tbb, PB[pk], B_cs[0], B_cs[1], B_ob[k2])
                            which, h0 = dest
                            transpose_out(ob[k2], B_ob[k2], k2, qT3 if which == "q" else KT3, h0, 4, tg, B_q if which == "q" else B_KT)
                        elif kind == "v":
                            av, h0 = dest
                            Sx.act("activation", out=ob[k2], in_=psf(pk), func=AF.Copy, r=[PB[pk]], w=[B_ob[k2]])
                            W([B_V], Sx.dma("pool", "dma_start", out=V4[av * 8 + h0:av * 8 + h0 + 4, :, tg // 128, :].rearrange("h p d -> p h d"),
                                            in_=ob[k2].rearrange("p (h d) -> p h d", h=4), r=[B_ob[k2]]))
                        else:
                            Sx.dve("tensor_copy", out=kit[:, 0:64], in_=psf(pk)[:, 0:64], r=[PB[pk]], w=[B_kit])
                            ln_inplace(kit[:, 0:64], 64, B_kit, st8, B_st8, 0)
                            Sx.dve("tensor_tensor", out=kit[:, 0:64], in0=kit[:, 0:64], in1=lng64, op=ALU.mult, r=[B_kit, B_l64], w=[B_kit])
                            Sx.dve("tensor_tensor", out=kit[:, 0:64], in0=kit[:, 0:64], in1=lnb64, op=ALU.add, r=[B_kit, B_l64], w=[B_kit])
                            k3 = kit[:, 0:64].rearrange("p (h d) -> p h d", h=1)
                            o3 = ob[k2][:, 0:64].rearrange("p (h d) -> p h d", h=1)
                            rope_ops(k3, o3, rope3[:, i, 128:160].unsqueeze(1), rope3[:, i, 160:192].unsqueeze(1), 32,
                                     cs[0][:, 0:32].rearrange("p (h d) -> p h d", h=1), cs[1][:, 0:32].rearrange("p (h d) -> p h d", h=1),
                                     B_kit, B_cs[0], B_cs[1], B_ob[k2])
                            W([B_ob[k2]], Sx.dve("tensor_copy", out=ob[k2][:, 64:128], in_=ob[k2][:, 0:64], r=[B_ob[k2]]))
                            transpose_out(ob[k2], B_ob[k2], k2, KT3, 16, 1, tg, B_KT)
                            sc_w = float(64 ** -0.5 * 16 ** -0.5)
                            Sx.act("activation", out=wi_t[:, 0:16], in_=psf(pk)[:, 64:80], func=AF.Abs, scale=sc_w, r=[PB[pk]], w=[B_wit])
                            W([B_wit], Sx.act("activation", out=wi_t[:, 16:32], in_=psf(pk)[:, 64:80], func=AF.Sign, r=[PB[pk]]))
                            W([B_wi], Sx.dma("pool", "dma_start", out=wi_d[tg:tg + 128, :], in_=wi_t, r=[B_wit]))
            else:
                gq, gkv = salloc(512), salloc(512)
                B_gn = Buf()
                Sx.dma("sp", "dma_start", out=gq, in_=mlan_in[jl * 2:jl * 2 + 1, :].to_broadcast([128, 512]), w=[B_gn])
                W([B_gn], Sx.dma("sp", "dma_start", out=gkv, in_=mlan_in[jl * 2 + 1:jl * 2 + 2, :].to_broadcast([128, 512])))
                cqT = salloc(4 * TG, BF16)
                cqT3 = cqT.rearrange("p (k t) -> p k t", k=4)
                B_cqT = Buf()
                xn = [salloc(512) for _ in range(2)]
                B_xn = [Buf(), Buf()]
                st8 = salloc(8)
                B_st8 = Buf()
                wsrc = mdown_b[jl * D:(jl + 1) * D, :]
                for ci, (c0, ncol, kind) in enumerate([(0, 512, "cq"), (512, 512, "ckv"), (1024, 64, "kr")]):
                    wk = ci % 2
                    w3 = wch[wk].rearrange("p (k n) -> p k n", k=KC)
                    Sx.dma("sp", "dma_start", out=w3[:, :, 0:ncol], in_=wsrc[:, c0:c0 + ncol].rearrange("(k p) n -> p k n", p=128),
                           r=Bw(("wdown", l)), w=[B_wch[wk]])
                    for i in range(NTG):
                        pk = cnt % 2
                        k2 = cnt % 2
                        cnt += 1
                        tg = t0 + i * 128
                        for kc in range(KC):
                            Sx.pe("matmul", psf(pk)[:, 0:ncol], lhsT=hT3[:, kc, i * 128:(i + 1) * 128], rhs=w3[:, kc, 0:ncol],
                                  start=(kc == 0), stop=(kc == KC - 1), r=[B_hT, B_wch[wk]], w=[PB[pk]])
                        if kind in ("cq", "ckv"):
                            gg = gq if kind == "cq" else gkv
                            Sx.act("activation", out=xn[k2], in_=psf(pk), func=AF.Square, accum_out=st8[:, 0:1], r=[PB[pk]], w=[B_xn[k2], B_st8])
                            Sx.act("activation", out=st8[:, 1:2], in_=st8[:, 0:1], func=AF.Sqrt, bias=eps5[:, 1:2], scale=1.0 / 512.0, r=[B_st8, B_eps], w=[B_st8])
                            Sx.dve("reciprocal", out=st8[:, 2:3], in_=st8[:, 1:2], r=[B_st8], w=[B_st8])
                            Sx.dve("scalar_tensor_tensor", out=ob[k2], in0=psf(pk), scalar=st8[:, 2:3], in1=gg, op0=ALU.mult, op1=ALU.mult,
                                   r=[PB[pk], B_st8, B_gn, B_xn[k2]], w=[B_ob[k2]])
                            if kind == "ckv":
                                transpose_out(ob[k2], B_ob[k2], k2, KT3, 0, 4, tg, B_KT)
                            else:
                                tk = 4 + k2
                                for q in range(4):
                                    Sx.pe("transpose", out=psh(tk)[:, q * 128:(q + 1) * 128], in_=ob[k2][:, q * 128:(q + 1) * 128], identity=ident_b,
                                          r=[B_ob[k2], B_identb], w=[PB[tk]])
                                W([B_cqT], Sx.act("activation", out=cqT3[:, :, i * 128:(i + 1) * 128], in_=psh(tk)[:, 0:512].rearrange("p (q t) -> p q t", q=4),
                                                  func=AF.Copy, r=[PB[tk]]))
                        else:
                            p3 = psf(pk)[:, 0:64].rearrange("p (h d) -> p h d", h=1)
                            o3 = ob[k2][:, 0:64].rearrange("p (h d) -> p h d", h=1)
                            rope_ops(p3, o3, rope3[:, i, 128:160].unsqueeze(1), rope3[:, i, 160:192].unsqueeze(1), 32,
                                     cs[0][:, 0:32].rearrange("p (h d) -> p h d", h=1), cs[1][:, 0:32].rearrange("p (h d) -> p h d", h=1),
                                     PB[pk], B_cs[0], B_cs[1], B_ob[k2])
                            W([B_ob[k2]], Sx.dve("tensor_copy", out=ob[k2][:, 64:128], in_=ob[k2][:, 0:64], r=[B_ob[k2]]))
                            transpose_out(ob[k2], B_ob[k2], k2, KT3, 4, 1, tg, B_KT)
                wq = salloc(4 * 3072, BF16)
                wq3 = wq.rearrange("p (k n) -> p k n", k=4)
                B_wq = Buf()
                Sx.dma("sp", "dma_start", out=wq3, in_=mqup_b[jl * 512:(jl + 1) * 512, :].rearrange("(k p) n -> p k n", p=128), r=Bw(("wqup", l)), w=[B_wq])
                for h in range(16):
                    for tp in range(0, TG, 512):
                        pk = cnt % 2
                        k2 = cnt % 2
                        cnt += 1
                        n = min(512, TG - tp)
                        for kc in range(4):
                            Sx.pe("matmul", psf(pk)[:, 0:n], lhsT=wq3[:, kc, h * 192:h * 192 + 128], rhs=cqT3[:, kc, tp:tp + n],
                                  start=(kc == 0), stop=(kc == 3), r=[B_wq, B_cqT], w=[PB[pk]])
                        Sx.act("activation", out=ob[k2][:, 0:n], in_=psf(pk)[:, 0:n], func=AF.Copy, r=[PB[pk]], w=[B_ob[k2]])
                        W([B_q], Sx.dma("pool", "dma_start", out=qT3[h, :, t0 + tp:t0 + tp + n], in_=ob[k2][:, 0:n], r=[B_ob[k2]]))
                wq4 = wq.rearrange("p (k h n) -> p k h n", k=4, h=16)
                for i in range(NTG):
                    tg = t0 + i * 128
                    for hh in range(2):
                        pk = cnt % 2
                        k2 = cnt % 2
                        cnt += 1
                        for kc in range(4):
                            Sx.pe("matmul", psf(pk).rearrange("p (h d) -> p h d", h=8), lhsT=cqT3[:, kc, i * 128:(i + 1) * 128],
                                  rhs=wq4[:, kc, hh * 8:(hh + 1) * 8, 128:192], start=(kc == 0), stop=(kc == 3), r=[B_wq, B_cqT], w=[PB[pk]])
                        cosb = rope3[:, i, 128:160].unsqueeze(1).to_broadcast([128, 8, 32])
                        sinb = rope3[:, i, 160:192].unsqueeze(1).to_broadcast([128, 8, 32])
                        rope_ops(psf(pk).rearrange("p (h d) -> p h d", h=8), ob[k2].rearrange("p (h d) -> p h d", h=8), cosb, sinb, 32,
                                 cs[0].rearrange("p (h d) -> p h d", h=8), cs[1].rearrange("p (h d) -> p h d", h=8), PB[pk], B_cs[0], B_cs[1], B_ob[k2])
                        transpose_out(ob[k2], B_ob[k2], k2, qT3, 16 + hh * 4, 4, tg, B_q)
            srelease(mL)
            Sx.barrier()
        if KSTOP == "P":
            break

        mA = smark()
        a = attn_alloc()
        aot = [salloc(2048, BF16) for _ in range(2)]
        B_aot = [Buf(), Buf()]
        nkvb = 2 if even else 1
        Kb = [salloc(S, BF16) for _ in range(nkvb)]
        B_Kb = [Buf() for _ in range(nkvb)]
        Vb = [salloc(S, BF16) for _ in range(nkvb)]
        B_Vb = [Buf() for _ in range(nkvb)]
        qb_ = [salloc(128, BF16) for _ in range(2)]
        B_qb = [Buf(), Buf()]
        kvc = [0]

        def load_KV(hk, hv, nk, nkt):
            k = kvc[0] % 2
            kvc[0] += 1
            if hk is not None:
                Sx.dma("sp", "dma_start", out=Kb[k][:, 0:nk], in_=KT3[hk, :, 0:nk], r=[B_KT], w=[B_Kb[k]])
            if hv is not None:
                Sx.dma("sp", "dma_start", out=Vb[k][:, 0:nkt * 128], in_=V_d[hv * 128:(hv + 1) * 128, 0:nkt * 128], r=[B_V], w=[B_Vb[k]])
            return k

        if even:
            kmean = salloc(8 * NB, BF16)
            km3 = kmean.rearrange("p (h n) -> p h n", h=8)
            kmf = salloc(NB)
            B_km, B_kmf = Buf(), Buf()
            for h in range(8):
                k = load_KV(8 + h, None, S, 0)
                Sx.dve("tensor_reduce", out=kmf, in_=Kb[k].rearrange("p (n k) -> p n k", k=256), axis=AX.X, op=ALU.add, r=[B_Kb[k]], w=[B_kmf])
                W([B_km], Sx.dve("tensor_scalar", out=km3[:, h, :], in0=kmf, scalar1=1.0 / 256.0, scalar2=None, op0=ALU.mult, r=[B_kmf]))
            kiT = salloc(S, BF16)
            B_kiT = Buf()
            Sx.dma("sp", "dma_start", out=kiT, in_=KT3[16, :, :], r=[B_KT], w=[B_kiT])
            work = salloc(S)
            B_work = Buf()
            biasb = work.bitcast(BF16)[:, 0:S]
            qi_t = salloc(8 * 128, BF16)
            qi3 = qi_t.rearrange("p (h t) -> p h t", h=8)
            B_qi = Buf()
            wi_s = salloc(32)
            B_wis = Buf()
            rl = [salloc(512) for _ in range(2)]
            B_rl = [Buf(), Buf()]
            m8 = salloc(8)
            thr = salloc(2)
            B_m8, B_thr = Buf(), Buf()
            gmo = salloc(2 * NB)
            B_gmo = Buf()
            NBP = max(NB, 8)
            gt = salloc(NBP)
            bbt = salloc(NBP)
            B_gt, B_bb = Buf(), Buf()
            Sx.dve("memset", gt, -3e38, w=[B_gt])
            acc2 = a.s
            B_acc = a.B_s
            ic = 0
            for i in range(NT):
                nch = i // 4 + 1
                r = i % 4
                nk = 128 * (i + 1)
                t0 = i * 128
                ka = i % 2
                Sx.dma("sp", "dma_start", out=qi3, in_=qT3[16:24, :, t0:t0 + 128].rearrange("h p t -> p h t"), r=[B_q], w=[B_qi])
                Sx.dma("sp", "dma_start", out=wi_s, in_=wi_d[t0:t0 + 128, :], r=[B_wi], w=[B_wis])
                for hh in range(16):
                    pr, hf = hh // 2, hh % 2
                    for m in range(nch):
                        n = 512 if m < nch - 1 else 128 * (r + 1)
                        pk = [0, 1, 6, 7][ic % 4]
                        k2 = ic % 2
                        ic += 1
                        Sx.pe("matmul", psf(pk)[:, 0:n], lhsT=qi3[hf * 64:(hf + 1) * 64, pr, :], rhs=kiT[hf * 64:(hf + 1) * 64, m * 512:m * 512 + n],
                              start=True, stop=True, r=[B_qi, B_kiT], w=[PB[pk]])
                        Sx.act("activation", out=rl[k2][:, 0:n], in_=psf(pk)[:, 0:n], func=AF.Relu, scale=wi_s[:, hh:hh + 1], r=[PB[pk], B_wis], w=[B_rl[k2]])
                        dst = acc2[:, m * 512:m * 512 + n]
                        if hh == 0:
                            op = Sx.dve("tensor_scalar", out=dst, in0=rl[k2][:, 0:n], scalar1=wi_s[:, 16:17], scalar2=None, op0=ALU.mult,
                                        r=[B_rl[k2], B_wis], w=[B_acc] if m == 0 else [])
                            if m:
                                W([B_acc], op)
                        else:
                            op = Sx.dve("scalar_tensor_tensor", out=dst, in0=rl[k2][:, 0:n], scalar=wi_s[:, 16 + hh:17 + hh], in1=dst, op0=ALU.mult, op1=ALU.add,
                                        r=[B_rl[k2], B_wis, B_acc], w=[])
                            W([B_acc], op)
                nl = 128 * (r + 1)
                lo = (nch - 1) * 512
                Sx.dve("tensor_tensor", out=acc2[:, lo:lo + nl], in0=acc2[:, lo:lo + nl], in1=cmask3[:, r, 0:nl], op=ALU.add, r=[B_acc, B_const], w=[B_acc])
                rounds = c.topk // 8
                if nk <= c.topk:
                    Sx.dve("memset", thr[:, 0:1], -1e29, w=[B_thr])
                else:
                    src = acc2
                    B_src = B_acc
                    for rd in range(rounds):
                        Sx.dve("max", out=m8, in_=src[:, 0:nk], r=[B_src], w=[B_m8])
                        if rd < rounds - 1:
                            Sx.dve("match_replace", out=work[:, 0:nk], in_to_replace=m8, in_values=src[:, 0:nk], imm_value=-3e38, r=[B_src, B_m8], w=[B_work])
                            src, B_src = work, B_work
                    Sx.dve("tensor_scalar", out=thr[:, 0:1], in0=m8[:, 7:8], scalar1=-1e29, scalar2=None, op0=ALU.max, r=[B_m8], w=[B_thr])
                Sx.dve("tensor_scalar", out=biasb[:, 0:nk], in0=acc2[:, 0:nk], scalar1=thr[:, 0:1], scalar2=NEG, op0=ALU.is_lt, op1=ALU.mult,
                       r=[B_acc, B_thr, B_work], w=[B_work])
                for h in range(8):
                    k = load_KV(h, h, nk, i + 1)
                    kq = (i * 16 + h) % 2
                    Sx.dma("sp", "dma_start", out=qb_[kq], in_=qT3[h, :, t0:t0 + 128], r=[B_q], w=[B_qb[kq]])
                    V3 = Vb[k].rearrange("p (i d) -> p i d", d=128)
                    Kt = Kb[k]
                    attn_core(a, i, [(qb_[kq], (lambda c0, n, Kt=Kt: Kt[:, c0:c0 + n]), [B_qb[kq], B_Kb[k]])], V3, B_Vb[k], float(128 ** -0.5), "dsa",
                              aot[ka][:, h * 128:(h + 1) * 128], B_aot[ka], bias=biasb, B_bias=B_work)
                Sx.dma("sp", "dma_start", out=gmo, in_=gmo_in[i:i + 1, :].to_broadcast([128, 2 * NB]), w=[B_gmo])
                for h in range(8):
                    k = load_KV(8 + h, 8 + h, nk, i + 1)
                    kq = (i * 16 + 8 + h) % 2
                    Sx.dma("sp", "dma_start", out=qb_[kq], in_=qT3[8 + h, :, t0:t0 + 128], r=[B_q], w=[B_qb[kq]])
                    Sx.pe("matmul", psf(6)[:, 0:NB], lhsT=qb_[kq], rhs=km3[:, h, :], start=True, stop=True, r=[B_qb[kq], B_km], w=[PB[6]])
                    Sx.dve("tensor_tensor", out=gt[:, 0:NB], in0=psf(6)[:, 0:NB], in1=gmo[:, 0:NB], op=ALU.add, r=[PB[6], B_gmo, B_gt], w=[B_gt])
                    Sx.dve("max", out=m8, in_=gt, r=[B_gt], w=[B_m8])
                    Sx.dve("tensor_scalar", out=thr[:, 1:2], in0=m8[:, c.topb - 1:c.topb], scalar1=-1e29, scalar2=None, op0=ALU.max, r=[B_m8], w=[B_thr])
                    Sx.dve("tensor_scalar", out=bbt[:, 0:NB], in0=gt[:, 0:NB], scalar1=thr[:, 1:2], scalar2=None, op0=ALU.is_ge, r=[B_gt, B_thr], w=[B_bb])
                    Sx.dve("tensor_tensor", out=bbt[:, 0:NB], in0=bbt[:, 0:NB], in1=gmo[:, NB:2 * NB], op=ALU.max, r=[B_bb, B_gmo], w=[B_bb])
                    Sx.dve("tensor_scalar", out=bbt[:, 0:NB], in0=bbt[:, 0:NB], scalar1=-1.0, scalar2=1e30, op0=ALU.add, op1=ALU.mult, r=[B_bb], w=[B_bb])
                    V3 = Vb[k].rearrange("p (i d) -> p i d", d=128)
                    Kt = Kb[k]
                    attn_core(a, i, [(qb_[kq], (lambda c0, n, Kt=Kt: Kt[:, c0:c0 + n]), [B_qb[kq], B_Kb[k]])], V3, B_Vb[k], float(128 ** -0.5), "moba",
                              aot[ka][:, (8 + h) * 128:(9 + h) * 128], B_aot[ka], bb=bbt, B_bb=B_bb)
                W([B_ao], Sx.dma("pool", "dma_start", out=ao_d[t0:t0 + 128, :], in_=aot[ka], r=[B_aot[ka]]))
        else:
            ckvT = salloc(4 * S, BF16)
            ckv3 = ckvT.rearrange("p (k t) -> p k t", k=4)
            krT = salloc(S, BF16)
            B_ckv, B_kr = Buf(), Buf()
            Sx.dma("sp", "dma_start", out=ckv3, in_=KT3[0:4, :, :].rearrange("h p t -> p h t"), r=[B_KT], w=[B_ckv])
            Sx.dma("sp", "dma_start", out=krT, in_=KT3[4, :, :], r=[B_KT], w=[B_kr])
            wkv = salloc(4 * 256, BF16)
            wkv3 = wkv.rearrange("p (k n) -> p k n", k=4)
            B_wkv = Buf()
            qn = [salloc(128, BF16) for _ in range(2)]
            qr = [salloc(128, BF16) for _ in range(2)]
            B_qn, B_qr = [Buf(), Buf()], [Buf(), Buf()]
            ot = [salloc(128, BF16) for _ in range(2)]
            B_ot = [Buf(), Buf()]
            cc = 0
            for h in range(16):
                Sx.dma("sp", "dma_start", out=wkv3, in_=mkvup_b[jl * 512:(jl + 1) * 512, h * 256:(h + 1) * 256].rearrange("(k p) n -> p k n", p=128),
                       r=Bw(("wkvup", l)), w=[B_wkv])
                k = 0
                Kt = Kb[k]
                V3 = Vb[k].rearrange("p (i d) -> p i d", d=128)
                for tp in range(0, S, 512):
                    pk = cc % 2
                    cc += 1
                    for kc in range(4):
                        Sx.pe("matmul", psf(pk), lhsT=wkv3[:, kc, 0:128], rhs=ckv3[:, kc, tp:tp + 512], start=(kc == 0), stop=(kc == 3), r=[B_wkv, B_ckv], w=[PB[pk]])
                    op = Sx.act("activation", out=Kt[:, tp:tp + 512], in_=psf(pk), func=AF.Copy, r=[PB[pk]], w=[B_Kb[k]] if tp == 0 else [])
                    if tp:
                        W([B_Kb[k]], op)
                for t4 in range(0, NT, 4):
                    pk = cc % 2
                    cc += 1
                    for q in range(4):
                        for kc in range(4):
                            Sx.pe("matmul", psf(pk)[:, q * 128:(q + 1) * 128], lhsT=ckv3[:, kc, (t4 + q) * 128:(t4 + q + 1) * 128], rhs=wkv3[:, kc, 128:256],
                                  start=(kc == 0), stop=(kc == 3), r=[B_wkv, B_ckv], w=[PB[pk]])
                    op = Sx.act("activation", out=Vb[k][:, t4 * 128:(t4 + 4) * 128], in_=psf(pk), func=AF.Copy, r=[PB[pk]], w=[B_Vb[k]] if t4 == 0 else [])
                    if t4:
                        W([B_Vb[k]], op)
                hf = h % 2
                for i in range(NT):
                    t0 = i * 128
                    ko = (h * NT + i) % 2
                    Sx.dma("sp", "dma_start", out=qn[ko], in_=qT3[h, :, t0:t0 + 128], r=[B_q], w=[B_qn[ko]])
                    Sx.dma("sp", "dma_start", out=qr[ko], in_=qT3[16 + h // 2, :, t0:t0 + 128], r=[B_q], w=[B_qr[ko]])
                    parts = [(qn[ko], (lambda c0, n, Kt=Kt: Kt[:, c0:c0 + n]), [B_qn[ko], B_Kb[k]]),
                             (qr[ko][hf * 64:(hf + 1) * 64, :], (lambda c0, n, hf=hf: krT[hf * 64:(hf + 1) * 64, c0:c0 + n]), [B_qr[ko], B_kr])]
                    attn_core(a, i, parts, V3, B_Vb[k], float(192 ** -0.5), "mla", ot[ko], B_ot[ko])
                    W([B_ao], Sx.dma("pool", "dma_start", out=ao_d[t0:t0 + 128, h * 128:(h + 1) * 128], in_=ot[ko], r=[B_ot[ko]]))
        srelease(mA)
        Sx.barrier()
        if KSTOP == "A":
            break

        mO = smark()
        wo = salloc(16 * D, BF16)
        wo3 = wo.rearrange("p (k n) -> p k n", k=16)
        B_wo = Buf()
        wout_b = ewout_b if even else mwout_b
        Sx.dma("sp", "dma_start", out=wo3, in_=wout_b[jl * 2048:(jl + 1) * 2048, :].rearrange("(k p) n -> p k n", p=128), r=Bw(("wout", l)), w=[B_wo])
        prm = residual_prm(l, 0)
        aob = [salloc(2048, BF16) for _ in range(2)]
        B_aob = [Buf(), Buf()]
        aoT = [salloc(2048, BF16) for _ in range(2)]
        B_aoT = [Buf(), Buf()]

        def y_attn(i, ybuf, B_y):
            k = i % 2
            Sx.dma("sp", "dma_start", out=aob[k], in_=ao_d[i * 128:(i + 1) * 128, :], r=[B_ao], w=[B_aob[k]])
            for g in range(2):
                tk = 6 + g
                for q in range(8):
                    Sx.pe("transpose", out=psh(tk)[:, q * 128:(q + 1) * 128], in_=aob[k][:, (g * 8 + q) * 128:(g * 8 + q + 1) * 128], identity=ident_b,
                          r=[B_aob[k], B_identb], w=[PB[tk]])
                op = Sx.act("activation", out=aoT[k][:, g * 1024:(g + 1) * 1024], in_=psh(tk), func=AF.Copy, r=[PB[tk]], w=[B_aoT[k]] if g == 0 else [])
                if g:
                    W([B_aoT[k]], op)
            for d0 in range(0, D, 512):
                pk = (d0 // 512) % 4
                for kc in range(16):
                    Sx.pe("matmul", psf(pk), lhsT=aoT[k][:, kc * 128:(kc + 1) * 128], rhs=wo3[:, kc, d0:d0 + 512], start=(kc == 0), stop=(kc == 15),
                          r=[B_aoT[k], B_wo], w=[PB[pk]])
                op = Sx.act("activation", out=ybuf[:, d0:d0 + 512], in_=psf(pk), func=AF.Copy, r=[PB[pk]], w=[B_y] if d0 == 0 else [])
                if d0:
                    W([B_y], op)

        residual_ln(l, 0, x_cur, B_x, xmid, B_xmid, 0, NT, y_attn, prm)
        srelease(mO)
        Sx.barrier()
        if KSTOP == "O":
            break

        x_new = y_out if last else xres
        EG = c.EG
        NTE = EG // 128
        for g in range(S // EG):
            t0 = g * EG
            mE = smark()
            hT = salloc(KC * EG, BF16)
            hT3 = hT.rearrange("p (k t) -> p k t", k=KC)
            B_hT = Buf()
            combT = salloc(EG, BF16)
            B_combT = Buf()
            selm = salloc(NE * 128, BF16)
            B_sel = Buf()
            Sx.dma("sp", "dma_start", out=selm[0:NE, :], in_=sel_in, w=[B_sel])
            sel3 = selm.rearrange("p (e t) -> p e t", e=NE)
            bdn_b = salloc(D, BF16)
            B_bdn = Buf()
            Sx.dma("pool", "dma_start", out=bdn_b[0:NE, :], in_=bdn_in[l * NE:(l + 1) * NE, :], w=[B_bdn])
            bguT = salloc(NE * 8)
            B_bgu = Buf()
            bg3 = bguT.rearrange("p (e c) -> p e c", c=8)
            yacc = salloc(NTE * D)
            ya3 = yacc.rearrange("p (t d) -> p t d", d=D)
            B_ya = [Buf() for _ in range(NTE)]
            mR = smark()
            bgr = salloc(128)
            B_bgr = Buf()
            for q0 in range(0, NE * 8, 128):
                nq = min(128, NE * 8 - q0)
                Sx.dma("sp", "dma_start", out=bgr[0:nq, :], in_=bgu_in[l * NE * 8 + q0:l * NE * 8 + q0 + nq, :], w=[B_bgr])
                Sx.pe("transpose", out=psf(0)[:, 0:nq], in_=bgr[0:nq, :], identity=ident_f[0:nq, 0:nq], r=[B_bgr, B_const], w=[PB[0]])
                W([B_bgu], Sx.dve("tensor_copy", out=bguT[:, q0:q0 + nq], in_=psf(0)[:, 0:nq], r=[PB[0]]))
            Sx.dve("tensor_scalar", out=bg3[:, :, 4:8], in0=bg3[:, :, 4:8], scalar1=1.0, scalar2=None, op0=ALU.add, r=[B_bgu], w=[B_bgu])
            rw = salloc(KC * NE)
            rw3 = rw.rearrange("p (k e) -> p k e", k=KC)
            rbv = salloc(NE)
            B_rw = Buf()
            Sx.dma("sp", "dma_start", out=rw3, in_=rw_in[l * D:(l + 1) * D, :].rearrange("(k p) e -> p k e", p=128), w=[B_rw])
            W([B_rw], Sx.dma("sp", "dma_start", out=rbv, in_=rb_in[l:l + 1, :].to_broadcast([128, NE])))
            hTf = salloc(KC * 128)
            hTf3 = hTf.rearrange("p (k t) -> p k t", k=KC)
            B_hTf = Buf()
            NEP = max(NE, 8)
            lg = salloc(NEP)
            ex = salloc(NEP)
            cmb = salloc(NEP)
            m8 = salloc(8)
            sm = salloc(4)
            B_lg, B_m8r, B_smr, B_cmb = Buf(), Buf(), Buf(), Buf()
            Sx.dve("memset", lg, -3e38, w=[B_lg])

            def router_hook(i, xt, B_xt):
                for gq in range(KC // 4):
                    pk = 2 + (gq % 4)
                    for q in range(4):
                        kc = gq * 4 + q
                        Sx.pe("transpose", out=psf(pk)[:, q * 128:(q + 1) * 128], in_=xt[:, kc * 128:(kc + 1) * 128], identity=ident_f,
                              r=[B_xt, B_const], w=[PB[pk]])
                    op = Sx.dve("tensor_copy", out=hTf3[:, gq * 4:(gq + 1) * 4, :], in_=psf(pk).rearrange("p (q t) -> p q t", q=4), r=[PB[pk]],
                                w=[B_hTf] if gq == 0 else [])
                    if gq:
                        W([B_hTf], op)
                for kc in range(KC):
                    Sx.pe("matmul", psf(0)[:, 0:NE], lhsT=hTf3[:, kc, :], rhs=rw3[:, kc, :], start=(kc == 0), stop=(kc == KC - 1), r=[B_hTf, B_rw], w=[PB[0]])
                Sx.dve("tensor_tensor", out=lg[:, 0:NE], in0=psf(0)[:, 0:NE], in1=rbv, op=ALU.add, r=[PB[0], B_rw, B_lg], w=[B_lg])
                Sx.dve("max", out=m8, in_=lg, r=[B_lg], w=[B_m8r])
                Sx.dve("tensor_scalar", out=sm[:, 0:1], in0=m8[:, 0:1], scalar1=-1.0, scalar2=None, op0=ALU.mult, r=[B_m8r], w=[B_smr])
                Sx.act("activation", out=ex[:, 0:NE], in_=lg[:, 0:NE], func=AF.Exp, bias=sm[:, 0:1], scale=1.0, r=[B_lg, B_smr], w=[B_cmb])
                Sx.dve("scalar_tensor_tensor", out=ex[:, 0:NE], in0=lg[:, 0:NE], scalar=m8[:, 3:4], in1=ex[:, 0:NE], op0=ALU.is_ge, op1=ALU.mult,
                       r=[B_lg, B_m8r, B_cmb], w=[B_cmb])
                Sx.dve("reduce_sum", out=sm[:, 1:2], in_=ex[:, 0:NE], axis=AX.X, r=[B_cmb], w=[B_smr])
                Sx.dve("reciprocal", out=sm[:, 2:3], in_=sm[:, 1:2], r=[B_smr], w=[B_smr])
                Sx.dve("tensor_scalar", out=cmb[:, 0:NE], in0=ex[:, 0:NE], scalar1=sm[:, 2:3], scalar2=None, op0=ALU.mult, r=[B_cmb, B_smr], w=[B_cmb])
                Sx.pe("transpose", out=psf(1)[0:NE, 0:128], in_=cmb[:, 0:NE], identity=ident_f, r=[B_cmb, B_const], w=[PB[1]])
                W([B_combT], Sx.act("activation", out=combT[0:NE, i * 128:(i + 1) * 128], in_=psf(1)[0:NE, 0:128], func=AF.Copy, r=[PB[1]]))

            build_hT(l, 1, xmid, t0, NTE, hT3, B_hT, hook=router_hook, B_src=B_xmid)
            srelease(mR)
            Sx.barrier()
            mX = smark()
            wgu_t = [salloc(KC * 256, BF16) for _ in range(3)]
            B_wgu = [Buf() for _ in range(3)]
            wdn_t = [salloc(4 * D, BF16) for _ in range(2)]
            B_wdn = [Buf(), Buf()]
            actT = [salloc(4 * EG, BF16) for _ in range(2)]
            B_act = [Buf(), Buf()]
            glu = [salloc(512) for _ in range(2)]
            sig = [salloc(512) for _ in range(2)]
            lin = [salloc(512) for _ in range(2)]
            B_glu, B_sig, B_lin = [Buf(), Buf()], [Buf(), Buf()], [Buf(), Buf()]
            wc = 0
            ec = 0
            yc = 0
            for e in range(NE):
                ke = e % 2
                wd3 = wdn_t[ke].rearrange("p (k n) -> p k n", k=4)
                Sx.dma("sp", "dma_start", out=wdn_t[ke], in_=wdn_bl[l][e * 128:(e + 1) * 128, :], r=Bw(("wdn", l)), w=[B_wdn[ke]])
                a3 = actT[ke].rearrange("p (c t) -> p c t", c=4)
                for cch in range(4):
                    kw = wc % 3
                    wc += 1
                    wg3 = wgu_t[kw].rearrange("p (k n) -> p k n", k=KC)
                    Sx.dma("sp", "dma_start", out=wgu_t[kw], in_=wgu_bl[l][(e * 4 + cch) * 128:(e * 4 + cch + 1) * 128, :], r=Bw(("wgu", l)), w=[B_wgu[kw]])
                    for tp in range(0, EG, 512):
                        n = min(512, EG - tp)
                        k2 = ec % 2
                        ec += 1
                        pg, pl, pc = 0 + k2, 2 + k2, 4
                        for kc in range(KC):
                            Sx.pe("matmul", psf(pg)[:, 0:n], lhsT=wg3[:, kc, 0:128], rhs=hT3[:, kc, tp:tp + n], start=(kc == 0), stop=(kc == KC - 1),
                                  r=[B_wgu[kw], B_hT], w=[PB[pg]])
                        for kc in range(KC):
                            Sx.pe("matmul", psf(pl)[:, 0:n], lhsT=wg3[:, kc, 128:256], rhs=hT3[:, kc, tp:tp + n], start=(kc == 0), stop=(kc == KC - 1),
                                  r=[B_wgu[kw], B_hT], w=[PB[pl]])
                        Sx.pe("matmul", psf(pc)[:, 0:n], lhsT=sel3[0:NE, e, :], rhs=combT[0:NE, tp:tp + n], start=True, stop=True, r=[B_sel, B_combT], w=[PB[pc]])
                        Sx.dve("tensor_scalar", out=glu[k2][:, 0:n], in0=psf(pg)[:, 0:n], scalar1=bg3[:, e, cch:cch + 1], scalar2=7.0, op0=ALU.add, op1=ALU.min,
                               r=[PB[pg], B_bgu], w=[B_glu[k2]])
                        Sx.act("activation", out=sig[k2][:, 0:n], in_=glu[k2][:, 0:n], func=AF.Sigmoid, scale=1.702, r=[B_glu[k2]], w=[B_sig[k2]])
                        Sx.dve("tensor_scalar", out=lin[k2][:, 0:n], in0=psf(pl)[:, 0:n], scalar1=bg3[:, e, 4 + cch:5 + cch], scalar2=8.0, op0=ALU.add, op1=ALU.min,
                               r=[PB[pl], B_bgu], w=[B_lin[k2]])
                        Sx.dve("scalar_tensor_tensor", out=lin[k2][:, 0:n], in0=lin[k2][:, 0:n], scalar=-6.0, in1=glu[k2][:, 0:n], op0=ALU.max, op1=ALU.mult,
                               r=[B_lin[k2], B_glu[k2]], w=[B_lin[k2]])
                        Sx.pool("tensor_tensor", out=lin[k2][:, 0:n], in0=lin[k2][:, 0:n], in1=sig[k2][:, 0:n], op=ALU.mult, r=[B_lin[k2], B_sig[k2]], w=[B_lin[k2]])
                        W([B_act[ke]], Sx.dve("tensor_tensor", out=a3[:, cch, tp:tp + n], in0=lin[k2][:, 0:n], in1=psf(pc)[:, 0:n], op=ALU.mult,
                                              r=[B_lin[k2], PB[pc]]))
                for ti in range(NTE):
                    for d0 in range(0, D, 512):
                        py = 5 + (yc % 3)
                        yc += 1
                        for cch in range(4):
                            Sx.pe("matmul", psf(py), lhsT=a3[:, cch, ti * 128:(ti + 1) * 128], rhs=wd3[:, cch, d0:d0 + 512], start=(cch == 0),
                                  stop=(cch == 3 and e != 0), r=[B_act[ke], B_wdn[ke]], w=[PB[py]])
                        if e == 0:
                            Sx.pe("matmul", psf(py), lhsT=combT[0:NE, ti * 128:(ti + 1) * 128], rhs=bdn_b[0:NE, d0:d0 + 512], start=False, stop=True,
                                  r=[B_combT, B_bdn], w=[PB[py]])
                            op = Sx.act("activation", out=ya3[:, ti, d0:d0 + 512], in_=psf(py), func=AF.Copy, r=[PB[py]], w=[B_ya[ti]] if d0 == 0 else [])
                            if d0:
                                W([B_ya[ti]], op)
                        else:
                            Sx.dve("tensor_tensor", out=ya3[:, ti, d0:d0 + 512], in0=ya3[:, ti, d0:d0 + 512], in1=psf(py), op=ALU.add, r=[B_ya[ti], PB[py]], w=[B_ya[ti]])
            srelease(mX)
            Sx.barrier()
            prm = residual_prm(l, 1)

            def y_moe(i, ybuf, B_y):
                Sx.pool("tensor_copy", out=ybuf, in_=ya3[:, i, :], r=[B_ya[i]], w=[B_y])

            residual_ln(l, 1, xmid, B_xmid, x_new, B_xnew, t0, NTE, y_moe, prm)
            srelease(mE)
            Sx.barrier()
        x_cur = x_new
        B_x = B_xnew

    Sx.barrier()
    for name in dbg:
        ap, shape, dt = scr[name]
        o = nc.dram_tensor("dbg_" + name, shape, dt, kind="ExternalOutput").ap()
        nr, ncl = shape[0], shape[1]
        for r0 in range(0, nr, 128):
            for c0 in range(0, ncl, 4096):
                Sx.dma("sp", "dma_start", out=o[r0:min(nr, r0 + 128), c0:min(ncl, c0 + 4096)], in_=ap[r0:min(nr, r0 + 128), c0:min(ncl, c0 + 4096)])
    global LAST_OPS
    LAST_OPS = Sx.ops
    Sx.emit(block)
    srelease(0)
    es.close()
    return nc


def rope_np(pos, dim):
    inv = (1.0 / (np.float32(10000.0) ** (np.arange(0, dim, 2, dtype=np.float32) / np.float32(dim)))).astype(np.float32)
    ang = pos.astype(np.float32)[:, None] * inv[None, :]
    return np.cos(ang).astype(np.float32), np.sin(ang).astype(np.float32)


def prep_inputs(cfg, inp):
    c = cfg
    S, D, KC, NT, NE, DEPTH, NB = c.S, c.D, c.KC, c.NT, c.NE, c.DEPTH, c.NB
    f32 = np.float32

    def A(k):
        return np.ascontiguousarray(np.asarray(inp[k], f32))

    def fl(a, ncol):
        a = np.asarray(a, f32)
        if a.size == 0:
            return np.zeros((1, ncol), f32)
        return np.ascontiguousarray(a.reshape(-1, ncol))

    shared = {}
    for l in range(DEPTH):
        shared[f"w_mod{l}"] = np.ascontiguousarray(np.asarray(inp["w_mod"][l], f32))
    shared["b_mod"] = A("b_mod")
    shared["lnp"] = fl(np.stack([inp["ln1_g"], inp["ln1_b"], inp["ln2_g"], inp["ln2_b"]], axis=1), D)
    NEV, NOD = max(c.N_EVEN, 1), max(c.N_ODD, 1)

    def padl(a, n, shape):
        a = np.asarray(a, f32)
        if a.shape[0] == 0:
            if len(shape) == 2:
                return np.zeros((1, 1, shape[1]), f32)
            return np.zeros((n,) + shape, f32)
        return a

    shared["even_w_in"] = fl(padl(inp["even_w_in"], NEV, (D, c.EIN)), c.EIN)
    shared["even_w_out"] = fl(padl(inp["even_w_out"], NEV, (2048, D)), D)
    shared["idxln"] = fl(np.stack([padl(inp["idx_ln_g"], NEV, (64,)), padl(inp["idx_ln_b"], NEV, (64,))], axis=1), 64)
    shared["mla_w_down"] = fl(padl(inp["mla_w_down"], NOD, (D, 1088)), 1088)
    shared["mlan"] = fl(np.stack([padl(inp["mla_q_norm"], NOD, (512,)), padl(inp["mla_kv_norm"], NOD, (512,))], axis=1), 512)
    shared["mla_w_q_up"] = fl(padl(inp["mla_w_q_up"], NOD, (512, 3072)), 3072)
    shared["mla_w_kv_up"] = fl(padl(inp["mla_w_kv_up"], NOD, (512, 4096)), 4096)
    shared["mla_w_out"] = fl(padl(inp["mla_w_out"], NOD, (2048, D)), D)
    shared["rw"] = fl(inp["router_w"], NE)
    shared["rb"] = A("router_b")
    wgu = np.asarray(inp["exp_w_gu"], f32)
    wd_ = np.concatenate([wgu[..., 0::2], wgu[..., 1::2]], axis=-1)
    del wgu
    wd_ = wd_.reshape(DEPTH, NE, KC, 128, 2, 4, 128).transpose(0, 1, 5, 3, 2, 4, 6)
    for l in range(DEPTH):
        shared[f"wgu{l}"] = np.ascontiguousarray(wd_[l]).reshape(NE * 512, KC * 256)
    del wd_
    bgu = np.asarray(inp["exp_b_gu"], f32)
    shared["bgu"] = np.ascontiguousarray(np.concatenate([bgu[..., 0::2], bgu[..., 1::2]], axis=-1).reshape(-1, 128))
    wdn_ = np.asarray(inp["exp_w_down"], f32).reshape(DEPTH, NE, 4, 128, D).transpose(0, 1, 3, 2, 4)
    for l in range(DEPTH):
        shared[f"wdn{l}"] = np.ascontiguousarray(wdn_[l]).reshape(NE * 128, 4 * D)
    del wdn_
    shared["bdn"] = fl(inp["exp_b_down"], D)
    shared["ident"] = np.eye(128, dtype=f32)
    sel = np.zeros((NE, NE, 128), f32)
    for e in range(NE):
        sel[e, e, :] = 1.0
    shared["sel"] = sel.reshape(NE, NE * 128).astype(ml_dtypes.bfloat16)
    pos = np.arange(S)
    ch, sh = rope_np(pos, 128)
    ci, si = rope_np(pos, 64)
    shared["rope"] = np.ascontiguousarray(np.concatenate([ch, sh, ci, si], axis=1))
    tri = np.where(np.arange(128)[None, :] <= np.arange(128)[:, None], 0.0, NEG).astype(f32)
    cm = np.zeros((128, 4, 4, 128), f32)
    mm = np.zeros((128, 4, 4, 128), f32)
    for r in range(4):
        for t in range(4):
            if t == r:
                cm[:, r, t, :] = tri
            elif t > r:
                cm[:, r, t, :] = NEG
            if t // 2 == r // 2:
                if t == r:
                    mm[:, r, t, :] = tri
                elif t > r:
                    mm[:, r, t, :] = NEG
    shared["cmask"] = cm.reshape(128, 4 * 512)
    shared["mmoba"] = mm.reshape(128, 4 * 512)
    gmo = np.zeros((NT, 2, NB), f32)
    for i in range(NT):
        own = i // 2
        gmo[i, 0, own:] = NEG
        gmo[i, 1, own] = 1.0
    shared["gmo"] = gmo.reshape(NT, 2 * NB)
    x = np.asarray(inp["x"], f32)
    cc = np.asarray(inp["c"], f32)
    maps = []
    for b in range(NC_):
        m = dict(shared)
        m["x"] = np.ascontiguousarray(x[b])
        m["cT"] = np.ascontiguousarray(cc[b].reshape(KC, 128).T)
        maps.append(m)
    return maps


def cfg_from_inputs(inp, **kw):
    x = inp["x"]
    return Cfg(S=x.shape[1], D=x.shape[2], DEPTH=inp["w_mod"].shape[0], NE=inp["router_w"].shape[2], **kw)


def kernel_unfused(**inputs):
    full = cfg_from_inputs(inputs)
    DEPTH = full.DEPTH
    x_cur = np.asarray(inputs["x"], np.float32)
    progs = {}
    ne, no = 0, 0
    for l in range(DEPTH):
        par = l % 2
        cfg = Cfg(S=full.S, D=full.D, DEPTH=1, NE=full.NE, parity=par, alpha=full.alpha)
        sub = {"x": x_cur, "c": inputs["c"]}
        for k in ("w_mod", "b_mod", "ln1_g", "ln1_b", "ln2_g", "ln2_b", "router_w", "router_b", "exp_w_gu", "exp_b_gu", "exp_w_down", "exp_b_down"):
            sub[k] = np.asarray(inputs[k])[l:l + 1]
        j = l // 2
        for k in ("even_w_in", "even_w_out", "idx_ln_g", "idx_ln_b"):
            a = np.asarray(inputs[k])
            sub[k] = a[j:j + 1] if par == 0 else a[0:0]
        for k in ("mla_w_down", "mla_q_norm", "mla_kv_norm", "mla_w_q_up", "mla_w_kv_up", "mla_w_out"):
            a = np.asarray(inputs[k])
            sub[k] = a[j:j + 1] if par == 1 else a[0:0]
        maps = prep_inputs(cfg, sub)
        if par not in progs:
            progs[par] = build_program(cfg)
        res = run_bass_kernel_spmd(progs[par], maps, core_ids=list(range(NC_)))
        x_cur = np.stack([res.results[b]["y"] for b in range(NC_)], axis=0)
    return x_cur


def kernel(**inputs):
    cfg = cfg_from_inputs(inputs)
    maps = prep_inputs(cfg, inputs)
    nc = build_program(cfg)
    res = run_bass_kernel_spmd(nc, maps, core_ids=list(range(NC_)))
    return np.stack([res.results[b]["y"] for b in range(NC_)], axis=0)
```
